# Optimizing a Trainium2 kernel written in Bass

```python
import math
import jax, jax.numpy as jnp
from jax import lax
import numpy as np

D_MODEL = 2048
BATCH = 1
SEQ = 16384
DEPTH = 1

CHUNK = 64
EPS = 1e-6
H_A = 8
HD_A = 128
H_IDX = 16
D_IDX = 64
TOPK_MAX = 256
QBLOCK = 128
N_T5_BUCKETS = 32
T5_MAX_DIST = 1024
H_B = 8
HD_B = 128
N_LEFT_CHUNKS = 8
N_BAND = N_LEFT_CHUNKS + 1
REL_CLIP = 128
W_A = H_A * HD_A
W_B = H_B * HD_B
W_IDX_Q = H_IDX * D_IDX
SPLITS = [W_A, W_A, W_A, W_IDX_Q, D_IDX, H_IDX, W_B, W_B, W_B, D_MODEL, D_MODEL]
IN_COLS = sum(SPLITS)
N_GROUPS = 8
EXP_PER_GROUP = 8
N_EXPERTS = N_GROUPS * EXP_PER_GROUP
TOPK_IN_GROUP = 2
D_FF_E = D_MODEL // 4
MOE_BLOCK = 128

kernel_name = "hybrid_dsa_chunkband_hmoe_block"


def rmsnorm(x, g):
    xf = x.astype(jnp.float32)
    y = xf * lax.rsqrt(jnp.mean(xf * xf, axis=-1, keepdims=True) + EPS)
    return y.astype(x.dtype) * g


def t5_bucket(rel):
    nb = N_T5_BUCKETS // 2
    ret = (rel > 0).astype(jnp.int32) * nb
    n = jnp.abs(rel)
    max_exact = nb // 2
    nf = jnp.maximum(n, 1).astype(jnp.float32)
    large = max_exact + (jnp.log(nf / max_exact) / math.log(T5_MAX_DIST / max_exact)
                         * (nb - max_exact)).astype(jnp.int32)
    large = jnp.minimum(large, nb - 1)
    return ret + jnp.where(n < max_exact, n, large)


def dsa_branch(q, k, v, qi, ki, wi, t5_table):
    Bn, S = q.shape[0], q.shape[1]
    n_sel = min(TOPK_MAX, S // 4)
    nqb = S // QBLOCK
    key_pos = jnp.arange(S, dtype=jnp.int32)
    scale = 1.0 / math.sqrt(HD_A)
    w_scale = (H_IDX ** -0.5) * (D_IDX ** -0.5)

    def block(b):
        start = b * QBLOCK
        q_b = lax.dynamic_slice_in_dim(q, start, QBLOCK, axis=1)
        qi_b = lax.dynamic_slice_in_dim(qi, start, QBLOCK, axis=1)
        wi_b = lax.dynamic_slice_in_dim(wi, start, QBLOCK, axis=1)
        t = start + jnp.arange(QBLOCK, dtype=jnp.int32)
        limit = (t // CHUNK + 1) * CHUNK
        act = jax.nn.relu(jnp.einsum('bqhd,bsd->bqhs', qi_b, ki).astype(jnp.float32))
        score = jnp.einsum('bqhs,bqh->bqs', act, wi_b.astype(jnp.float32) * w_scale)
        vis = key_pos[None, :] < limit[:, None]
        score = jnp.where(vis[None], score, -jnp.inf)
        _, idx = lax.top_k(score, n_sel)
        valid = idx < limit[None, :, None]
        k_g = jax.vmap(lambda kk, ii: kk[ii])(k, idx)
        v_g = jax.vmap(lambda vv, ii: vv[ii])(v, idx)
        logits = jnp.einsum('bqhd,bqkhd->bhqk', q_b, k_g).astype(jnp.float32) * scale
        bias = t5_table[t5_bucket(idx - t[None, :, None])]
        logits = logits + jnp.transpose(bias, (0, 3, 1, 2)).astype(jnp.float32)
        logits = jnp.where(valid[:, None], logits, -jnp.inf)
        p = jax.nn.softmax(logits, axis=-1).astype(v.dtype)
        return jnp.einsum('bhqk,bqkhd->bqhd', p, v_g)

    out = lax.map(block, jnp.arange(nqb, dtype=jnp.int32))
    return jnp.transpose(out, (1, 0, 2, 3, 4)).reshape(Bn, S, W_A)


def chunk_band_branch(q, k, v, rel_table):
    Bn, S = q.shape[0], q.shape[1]
    nc = S // CHUNK
    pad = N_LEFT_CHUNKS * CHUNK
    kp = jnp.pad(k, ((0, 0), (pad, 0), (0, 0), (0, 0))).reshape(Bn, nc + N_LEFT_CHUNKS, CHUNK, H_B, HD_B)
    vp = jnp.pad(v, ((0, 0), (pad, 0), (0, 0), (0, 0))).reshape(Bn, nc + N_LEFT_CHUNKS, CHUNK, H_B, HD_B)
    k_band = jnp.concatenate([kp[:, j:j + nc] for j in range(N_BAND)], axis=2)
    v_band = jnp.concatenate([vp[:, j:j + nc] for j in range(N_BAND)], axis=2)
    qc = q.reshape(Bn, nc, CHUNK, H_B, HD_B)
    logits = jnp.einsum('bcqhd,bckhd->bchqk', qc, k_band).astype(jnp.float32) / math.sqrt(HD_B)
    qi = jnp.arange(CHUNK, dtype=jnp.int32)
    kj = jnp.arange(N_BAND * CHUNK, dtype=jnp.int32)
    rel = qi[:, None] - (kj[None, :] - pad)
    bias = rel_table[:, jnp.clip(rel, -REL_CLIP, REL_CLIP) + REL_CLIP]
    key_chunk = jnp.arange(nc, dtype=jnp.int32)[:, None] - N_LEFT_CHUNKS + kj[None, :] // CHUNK
    valid = key_chunk >= 0
    logits = jnp.where(valid[None, :, None, None, :],
                       logits + bias[None, None].astype(jnp.float32), -jnp.inf)
    p = jax.nn.softmax(logits, axis=-1).astype(v.dtype)
    out = jnp.einsum('bchqk,bckhd->bcqhd', p, v_band)
    return out.reshape(Bn, S, W_B)


def hierarchical_moe(h, w_rg, b_rg, w_re, b_re, w1, w3, w2):
    Bn, S, D = h.shape
    T = Bn * S
    xt = h.reshape(T, D)
    g_logits = (xt @ w_rg).astype(jnp.float32) + b_rg.astype(jnp.float32)
    g_prob = jax.nn.softmax(g_logits, axis=-1)
    grp = jnp.argmax(g_logits, axis=-1).astype(jnp.int32)
    p_grp = jnp.take_along_axis(g_prob, grp[:, None], axis=-1)[:, 0]
    e_logits = ((xt @ w_re).astype(jnp.float32) + b_re.astype(jnp.float32)).reshape(T, N_GROUPS, EXP_PER_GROUP)
    e_in = jnp.take_along_axis(e_logits, grp[:, None, None], axis=1)[:, 0]
    top_v, top_i = lax.top_k(e_in, TOPK_IN_GROUP)
    p_in = jax.nn.softmax(top_v, axis=-1)
    expert = grp[:, None] * EXP_PER_GROUP + top_i.astype(jnp.int32)
    weight = p_grp[:, None] * p_in

    M = T * TOPK_IN_GROUP
    e_flat = expert.reshape(M)
    tok_flat = jnp.repeat(jnp.arange(T, dtype=jnp.int32), TOPK_IN_GROUP)
    w_flat = weight.reshape(M)
    order = jnp.argsort(e_flat, stable=True)
    e_s, tok_s, w_s = e_flat[order], tok_flat[order], w_flat[order]
    counts = jnp.bincount(e_flat, length=N_EXPERTS).astype(jnp.int32)
    start = jnp.cumsum(counts) - counts
    padded = ((counts + MOE_BLOCK - 1) // MOE_BLOCK) * MOE_BLOCK
    pend = jnp.cumsum(padded)
    pstart = pend - padded
    dest = pstart[e_s] + (jnp.arange(M, dtype=jnp.int32) - start[e_s])
    nb = (M + MOE_BLOCK - 1) // MOE_BLOCK + N_EXPERTS
    P = nb * MOE_BLOCK
    tok_buf = jnp.zeros((P,), jnp.int32).at[dest].set(tok_s)
    w_buf = jnp.zeros((P,), h.dtype).at[dest].set(w_s.astype(h.dtype))
    blk_e = jnp.minimum(jnp.searchsorted(pend, jnp.arange(nb, dtype=jnp.int32) * MOE_BLOCK, side='right'),
                        N_EXPERTS - 1).astype(jnp.int32)

    def run(args):
        tok, wt, e = args
        xb = xt[tok]
        a = jax.nn.silu(xb @ w1[e]) * (xb @ w3[e])
        return (a @ w2[e]) * wt[:, None]

    y = lax.map(run, (tok_buf.reshape(nb, MOE_BLOCK), w_buf.reshape(nb, MOE_BLOCK), blk_e))
    out = jnp.zeros((T, D), h.dtype).at[tok_buf].add(y.reshape(P, D))
    return out.reshape(Bn, S, D)


def setup_inputs(seed: int = 0) -> dict:
    key = jax.random.key(seed)
    ks = jax.random.split(key, 24)
    f32 = jnp.float32
    D = D_MODEL

    def nrm(k, shape, scale):
        return jax.random.normal(k, shape, f32) * scale

    return {
        "x": nrm(ks[0], (BATCH, SEQ, D), 1.0),
        "c": nrm(ks[1], (BATCH, D), 1.0),
        "w_ada": nrm(ks[2], (DEPTH, D, 6 * D), D ** -0.5 * 0.5),
        "b_ada": nrm(ks[3], (DEPTH, 6 * D), 0.02),
        "norm_mix": 1.0 + nrm(ks[4], (DEPTH, D), 0.02),
        "w_in": nrm(ks[5], (DEPTH, D, IN_COLS), D ** -0.5),
        "t5_table": nrm(ks[6], (N_T5_BUCKETS, H_A), 0.3),
        "rel_table": nrm(ks[7], (DEPTH, H_B, 2 * REL_CLIP + 1), 0.3),
        "w_up_a": nrm(ks[8], (DEPTH, W_A, D), W_A ** -0.5),
        "w_up_b": nrm(ks[9], (DEPTH, W_B, D), W_B ** -0.5),
        "w_o": nrm(ks[10], (DEPTH, D, D), D ** -0.5),
        "norm_ffn": 1.0 + nrm(ks[11], (DEPTH, D), 0.02),
        "w_rg": nrm(ks[12], (DEPTH, D, N_GROUPS), D ** -0.5),
        "b_rg": nrm(ks[13], (DEPTH, N_GROUPS), 0.01),
        "w_re": nrm(ks[14], (DEPTH, D, N_EXPERTS), D ** -0.5),
        "b_re": nrm(ks[15], (DEPTH, N_EXPERTS), 0.01),
        "w1": nrm(ks[16], (DEPTH, N_EXPERTS, D, D_FF_E), D ** -0.5),
        "w3": nrm(ks[17], (DEPTH, N_EXPERTS, D, D_FF_E), D ** -0.5),
        "w2": nrm(ks[18], (DEPTH, N_EXPERTS, D_FF_E, D), D_FF_E ** -0.5),
        "norm_final": 1.0 + nrm(ks[19], (D,), 0.02),
    }


def reference(x, c, w_ada, b_ada, norm_mix, w_in, t5_table, rel_table, w_up_a, w_up_b,
              w_o, norm_ffn, w_rg, b_rg, w_re, b_re, w1, w3, w2, norm_final):
    Bn, S, D = x.shape
    split_pts = list(np.cumsum(SPLITS)[:-1])
    c_act = jax.nn.silu(c)
    for l in range(DEPTH):
        mod = c_act @ w_ada[l] + b_ada[l]
        sh1, sc1, g1, sh2, sc2, g2 = [m[:, None, :] for m in jnp.split(mod, 6, axis=-1)]

        h = rmsnorm(x, norm_mix[l]) * (1.0 + sc1) + sh1
        proj = h @ w_in[l]
        qa, ka, va, qi, ki, wi, qb, kb, vb, gate_a, gate_b = jnp.split(proj, split_pts, axis=-1)
        y_a = dsa_branch(qa.reshape(Bn, S, H_A, HD_A), ka.reshape(Bn, S, H_A, HD_A),
                         va.reshape(Bn, S, H_A, HD_A), qi.reshape(Bn, S, H_IDX, D_IDX),
                         ki, wi, t5_table)
        y_b = chunk_band_branch(qb.reshape(Bn, S, H_B, HD_B), kb.reshape(Bn, S, H_B, HD_B),
                                vb.reshape(Bn, S, H_B, HD_B), rel_table[l])
        merged = jax.nn.sigmoid(gate_a) * (y_a @ w_up_a[l]) + jax.nn.sigmoid(gate_b) * (y_b @ w_up_b[l])
        x = x + g1 * (merged @ w_o[l])

        h2 = rmsnorm(x, norm_ffn[l]) * (1.0 + sc2) + sh2
        x = x + g2 * hierarchical_moe(h2, w_rg[l], b_rg[l], w_re[l], b_re[l], w1[l], w3[l], w2[l])
    return rmsnorm(x, norm_final)
```

```python
import math
from contextlib import ExitStack

import numpy as np
import ml_dtypes

import concourse.bass as bass
import concourse.mybir as mybir
from concourse.bass_utils import run_bass_kernel_spmd

F32 = mybir.dt.float32
BF16 = mybir.dt.bfloat16
I32 = mybir.dt.int32
U32 = mybir.dt.uint32
ALU = mybir.AluOpType
AF = mybir.ActivationFunctionType
AX = mybir.AxisListType

D = 2048
S = 16384
NCORE = 8
TOK = 2048
NM = 16
KT = 16
EPS = 1e-6
C_QA, C_KA, C_VA, C_QI, C_KI, C_WI, C_QB, C_KB, C_VB, C_GA, C_GB = 0, 1024, 2048, 3072, 4096, 4160, 4176, 5200, 6224, 7248, 9296
NC_IN = 11344
NIT = 24
NEG = -30000.0
SCALE = 1.0 / math.sqrt(128.0)
CAP = 4
NBLK = 64 * CAP
NE = 64


class Tok:
    __slots__ = ("w", "r")

    def __init__(self):
        self.w = None
        self.r = {}


class KB:
    def __init__(self, nc, es):
        self.nc = nc
        self.es = es
        self.eng = {"pe": nc.tensor, "act": nc.scalar, "dve": nc.vector, "pool": nc.gpsimd, "sp": nc.sync}
        self.epoch = 0
        self._fresh()

    def _fresh(self):
        self.sem = {}
        self.cnt = {}
        self.seen = {k: {} for k in self.eng}
        for k in self.eng:
            self.sem[k] = self.es.enter_context(self.nc.semaphore("s%d_%s" % (self.epoch, k)))
            self.cnt[k] = 0

    def lane(self, name):
        L = "L" + name
        if L not in self.sem:
            self.sem[L] = self.es.enter_context(self.nc.semaphore("l%d_%s" % (self.epoch, name)))
            self.cnt[L] = 0
        return L

    def _deps(self, e, reads, writes):
        need = {}
        ep = self.epoch
        for t in reads:
            if t.w is not None and t.w[2] == ep:
                p, v, _ = t.w
                if v > need.get(p, 0):
                    need[p] = v
        for t in writes:
            if t.w is not None and t.w[2] == ep:
                p, v, _ = t.w
                if p != e and v > need.get(p, 0):
                    need[p] = v
            for p, (v, te) in t.r.items():
                if te == ep and p != e and v > need.get(p, 0):
                    need[p] = v
        seen = self.seen[e]
        for p, v in need.items():
            if seen.get(p, 0) < v:
                self.eng[e].wait_ge(self.sem[p], v)
                seen[p] = v

    def _rec(self, prod, val, reads, writes):
        for t in writes:
            t.w = (prod, val, self.epoch)
            t.r = {}
        for t in reads:
            t.r[prod] = (val, self.epoch)

    def op(self, e, fn, r=(), w=()):
        self._deps(e, r, w)
        ins = fn(self.eng[e])
        self.cnt[e] += 1
        assert self.cnt[e] < 60000
        ins.then_inc(self.sem[e], 1)
        self._rec(e, self.cnt[e], r, w)
        return ins

    def dma(self, q, lane, out, in_, r=(), w=(), **kw):
        L = self.lane(lane)
        self._deps(q, r, w)
        ins = self.eng[q].dma_start(out=out, in_=in_, **kw)
        self.cnt[L] += 16
        assert self.cnt[L] < 60000
        ins.then_inc(self.sem[L], 16)
        self._rec(L, self.cnt[L], r, w)
        return ins

    def custom(self, q, lane, fn, inc, r=(), w=(), throttle=4):
        L = self.lane(lane)
        self._deps(q, r, w)
        if throttle and inc == 16:
            tgt = self.cnt[L] - throttle * 16
            if tgt > 0 and self.seen[q].get(L, 0) < tgt:
                self.eng[q].wait_ge(self.sem[L], tgt)
                self.seen[q][L] = tgt
        ins = fn(self.eng[q])
        self.cnt[L] += inc
        assert self.cnt[L] < 60000
        ins.then_inc(self.sem[L], inc)
        self._rec(L, self.cnt[L], r, w)
        return ins

    def barrier(self, final=False):
        for e in self.eng:
            for p in self.sem:
                if p != e and self.cnt[p] > 0 and self.seen[e].get(p, 0) < self.cnt[p]:
                    self.eng[e].wait_ge(self.sem[p], self.cnt[p])
                    self.seen[e][p] = self.cnt[p]
        if not final:
            self.epoch += 1
            self._fresh()


def build(upto=99, dbg=(), p2test=False, SK=S, NBAND=80):
    nc = bass.Bass("TRN2", target_bir_lowering=False)

    def din(name, shape, dt=F32):
        if p2test and name not in ("ident_f", "ident_b", "vis_in", "t5c_in", "bbc_in", "cb_in"):
            shape = [1, 16]
        if upto <= 6 and name in ("w1_in", "w3_in", "w2_in"):
            shape = [1, 16]
        return nc.dram_tensor(name, list(shape), dt, kind="ExternalInput").ap()

    def dint(name, shape, dt=BF16):
        if p2test and name in ("QaT_d", "QiT_d", "QbT_d", "Wi_d", "K_abs", "V_abs", "kiT_abs", "KbT_loc", "Vb_loc"):
            return nc.dram_tensor(name, list(shape), dt, kind="ExternalInput").ap()
        return nc.dram_tensor(name, list(shape), dt, kind="Internal").ap()

    def dout(name, shape, dt=F32):
        return nc.dram_tensor(name, list(shape), dt, kind="ExternalOutput").ap()

    x_own = din("x_own", [TOK, D])
    x_all = din("x_all", [S, D])
    x_band = din("x_band", [80 * 128, D])
    ccol = din("ccol", [128, 16])
    w_ada = din("w_ada", [D, 6 * D])
    bada_col = din("bada_col", [128, 96])
    nmix_col = din("nmix_col", [128, 16])
    w_in = din("w_in", [D, NC_IN])
    ident_f = din("ident_f", [128, 128])
    ident_b = din("ident_b", [128, 128], BF16)
    vis_in = din("vis_in", [128, 1024])
    t5c_in = din("t5c_in", [8, 128, 16 * 128])
    bbc_in = din("bbc_in", [2, 8, 128, 5 * 128])
    cb_in = din("cb_in", [128, 8])
    w_up_a = din("w_up_a", [1024, D])
    w_up_b = din("w_up_b", [1024, D])
    w_o = din("w_o", [D, D])
    nffn_row = din("nffn_row", [1, D])
    nfin_row = din("nfin_row", [1, D])
    wr_in = din("wr_in", [D, 72])
    br_row = din("br_row", [1, 72])
    w1_in = din("w1_in", [NE * D, 512])
    w3_in = din("w3_in", [NE * D, 512])
    w2_in = din("w2_in", [NE * 512, D])
    U_in = din("U_in", [128, 128], BF16)
    ones_in = din("ones_in", [128, 128], BF16)
    Ue_in = din("Ue_in", [64, 64])
    base13_in = din("base13_in", [128, 16])
    base2_in = din("base2_in", [128, 4])
    estart_in = din("estart_in", [128, NE])
    y_out = dout("y_out", [TOK, D])
    X1_d = dint("X1_d", [TOK, D], F32)
    H2_d = dint("H2_d", [TOK, D], BF16)
    Xd = dint("Xd", [NBLK * 128, D], BF16)
    Yd = dint("Yd", [NBLK * 128, D], F32)
    Yd4 = Yd.rearrange("r (c f) -> (r c) f", f=512)

    QaT_d = dint("QaT_d", [1024, TOK])
    QiT_d = dint("QiT_d", [1024, TOK])
    QbT_d = dint("QbT_d", [1024, TOK])
    Wi_d = dint("Wi_d", [TOK, 16], F32)
    SGT_d = dint("SGT_d", [4096, TOK], F32)
    modflat = dint("modflat", [96, 128], F32)
    K_abs = dint("K_abs", [8, 128, SK])
    V_abs = dint("V_abs", [8, 128, SK])
    K_abs2 = K_abs.rearrange("h d s -> (h d) s")
    V_abs2 = V_abs.rearrange("h p x -> (h p) x")
    kiT_abs = dint("kiT_abs", [64, S])
    KbT_loc = dint("KbT_loc", [1024, NBAND * 128])
    Vb_loc = dint("Vb_loc", [1024, NBAND * 128])
    YaT_d = dint("YaT_d", [1024, TOK])
    YbT_d = dint("YbT_d", [1024, TOK])

    outs = {}
    for name, shape, dt in dbg:
        outs[name] = dout("o_" + name, shape, dt)

    with ExitStack() as es:
        kb = KB(nc, es)

        def sb(name, shape, dt, scope=es):
            return scope.enter_context(nc.sbuf_tensor(name, list(shape), dt))

        def ps(name, shape, dt, scope=es):
            return scope.enter_context(nc.psum_tensor(name, list(shape), dt))

        identf = sb("identf", [128, 128], F32)
        identb = sb("identb", [128, 128], BF16)
        cact = sb("cact", [128, 16], F32)
        modc = sb("modc", [128, 96], F32)
        A1 = sb("A1", [128, 16], F32)
        t_const = Tok()
        t_mod = Tok()
        kb.dma("sp", "c0", identf[:], ident_f, w=[t_const])
        kb.dma("sp", "c0", identb[:], ident_b, w=[t_const])

        bank = [ps("bank%d" % i, [128, 512], F32) for i in range(6)]
        bankb = [ps("bankb%d" % i, [128, 1024], BF16) for i in range(2)]
        t_bank = [Tok() for _ in range(6)]
        t_bankb = [Tok() for _ in range(2)]

        t_abs = Tok()
        t_modflat = Tok()
        if not p2test:
            with ExitStack() as p0:
                ccs = sb("ccs", [128, 16], F32, p0)
                bcs = sb("bcs", [128, 96], F32, p0)
                nms = sb("nms", [128, 16], F32, p0)
                wa = [sb("wa%d" % i, [128, 16, 512], F32, p0) for i in range(2)]
                t_wa = [Tok(), Tok()]
                t_cc = Tok()
                kb.dma("sp", "c1", ccs[:], ccol, w=[t_cc])
                kb.dma("sp", "c1", bcs[:], bada_col, w=[t_cc])
                kb.dma("sp", "c1", nms[:], nmix_col, w=[t_cc])
                kb.op("act", lambda e: e.activation(out=cact[:], in_=ccs[:], func=AF.Silu), r=[t_cc], w=[t_mod])
                wv = w_ada.rearrange("(k p) n -> p k n", p=128)
                for c in range(24):
                    s = c % 2
                    kb.dma("sp", "wa%d" % s, wa[s][:], wv[:, :, c * 512:(c + 1) * 512], w=[t_wa[s]])
                    for nt in range(4):
                        col = c * 4 + nt
                        for k in range(KT):
                            kb.op("pe", lambda e, s=s, nt=nt, k=k, col=col: e.matmul(
                                bank[0][:, col:col + 1], lhsT=wa[s][:, k, nt * 128:(nt + 1) * 128], rhs=cact[:, k:k + 1],
                                start=(k == 0), stop=(k == KT - 1)), r=[t_wa[s], t_mod], w=[t_bank[0]])
                kb.op("dve", lambda e: e.tensor_tensor(out=modc[:], in0=bank[0][:, 0:96], in1=bcs[:], op=ALU.add),
                      r=[t_bank[0], t_cc], w=[t_mod])
                kb.op("dve", lambda e: e.scalar_tensor_tensor(out=A1[:], in0=modc[:, 16:32], scalar=1.0, in1=nms[:],
                                                             op0=ALU.add, op1=ALU.mult), r=[t_mod, t_cc], w=[t_mod])
                mt = sb("mt", [96, 128], F32, p0)
                t_mt = Tok()
                kb.op("pe", lambda e: e.transpose(bank[1][0:96, 0:128], modc[:, 0:96], identf[:]), r=[t_mod, t_const], w=[t_bank[1]])
                kb.op("dve", lambda e: e.tensor_copy(out=mt[:], in_=bank[1][0:96, 0:128]), r=[t_bank[1]], w=[t_mt])
                t_modflat = Tok()
                kb.dma("sp", "c1", modflat, mt[:], r=[t_mt], w=[t_modflat])
                kb.barrier()
            if "modc" in outs:
                kb.dma("sp", "dbg", outs["modc"], modc[:], r=[t_mod], w=[Tok()])
            if upto <= 0:
                kb.barrier()
                return nc

            t_abs = Tok()
            with ExitStack() as p1:
                hT = sb("hT", [128, KT, TOK], BF16, p1)
                t_hT = [Tok() for _ in range(NM)]
                xb = [sb("xb%d" % i, [128, D], F32, p1) for i in range(2)]
                xn = [sb("xn%d" % i, [128, D], BF16, p1) for i in range(2)]
                ss = sb("ss", [128, 4], F32, p1)
                t_xb = [Tok(), Tok()]
                t_xn = [Tok(), Tok()]
                t_ss = [Tok(), Tok()]
                epsc = sb("epsc", [128, 1], F32, p1)
                t_eps = Tok()
                kb.op("dve", lambda e: e.memset(epsc[:], EPS), w=[t_eps])
                wc = [sb("wc%d" % i, [128, KT, 512], BF16, p1) for i in range(2)]
                t_wc = [Tok(), Tok()]
                stg_b = [sb("stgb%d" % i, [128, 512], BF16, p1) for i in range(4)]
                stg_f = [sb("stgf%d" % i, [128, 512], F32, p1) for i in range(4)]
                t_stg = [Tok() for _ in range(4)]
                winv = w_in.rearrange("(k p) n -> p k n", p=128)
                state = {"cnt": 0, "ci": 0}

                def proj_batches(xsrc, nb, chunks):
                    for bi in range(nb):
                        for lt in range(NM):
                            s = lt % 2
                            row = (bi * NM + lt) * 128
                            kb.dma("sp", "xb%d" % s, xb[s][:], xsrc[row:row + 128, :], w=[t_xb[s]])
                            kb.op("act", lambda e, s=s: e.activation(out=xn[s][:], in_=xb[s][:], func=AF.Square, accum_out=ss[:, s:s + 1]),
                                  r=[t_xb[s]], w=[t_xn[s], t_ss[s]])
                            kb.op("act", lambda e, s=s: e.activation(out=ss[:, s:s + 1], in_=ss[:, s:s + 1], func=AF.Sqrt, bias=epsc[:, 0:1], scale=1.0 / D),
                                  r=[t_ss[s], t_eps], w=[t_ss[s]])
                            kb.op("dve", lambda e, s=s: e.reciprocal(out=ss[:, 2 + s:3 + s], in_=ss[:, s:s + 1]), r=[t_ss[s]], w=[t_ss[s]])
                            kb.op("dve", lambda e, s=s: e.tensor_scalar(out=xn[s][:], in0=xb[s][:], scalar1=ss[:, 2 + s:3 + s], scalar2=None, op0=ALU.mult),
                                  r=[t_xb[s], t_ss[s]], w=[t_xn[s]])
                            for half in range(2):
                                for j in range(8):
                                    k = half * 8 + j
                                    kb.op("pe", lambda e, s=s, half=half, j=j, k=k: e.transpose(
                                        bankb[half][:, j * 128:(j + 1) * 128], xn[s][:, k * 128:(k + 1) * 128], identb[:]),
                                        r=[t_xn[s], t_const], w=[t_bankb[half]])
                                for j in range(8):
                                    k = half * 8 + j
                                    if j % 2 == 0:
                                        kb.op("dve", lambda e, half=half, j=j, k=k, lt=lt: e.tensor_scalar(
                                            out=hT[:, k, lt * 128:(lt + 1) * 128], in0=bankb[half][:, j * 128:(j + 1) * 128],
                                            scalar1=A1[:, k:k + 1], scalar2=modc[:, k:k + 1], op0=ALU.mult, op1=ALU.add),
                                            r=[t_bankb[half], t_mod], w=[t_hT[lt]])
                                    else:
                                        kb.op("act", lambda e, half=half, j=j, k=k, lt=lt: e.activation(
                                            out=hT[:, k, lt * 128:(lt + 1) * 128], in_=bankb[half][:, j * 128:(j + 1) * 128],
                                            func=AF.Identity, bias=modc[:, k:k + 1], scale=A1[:, k:k + 1]),
                                            r=[t_bankb[half], t_mod], w=[t_hT[lt]])
                        for (c0, ncol, kind, dest, row0) in chunks:
                            s = state["ci"] % 2
                            state["ci"] += 1
                            kb.dma("pool", "wc%d" % s, wc[s][:, :, 0:ncol], winv[:, :, c0:c0 + ncol], w=[t_wc[s]])
                            if kind in ("fm", "sg"):
                                for nt in range((ncol + 127) // 128):
                                    M = min(128, ncol - nt * 128)
                                    for g in range(4):
                                        cnt = state["cnt"]
                                        b = 2 + cnt % 4
                                        st = cnt % 4
                                        state["cnt"] += 1
                                        for k in range(KT):
                                            kb.op("pe", lambda e, s=s, nt=nt, M=M, g=g, b=b, k=k: e.matmul(
                                                bank[b][0:M, :], lhsT=wc[s][:, k, nt * 128:nt * 128 + M], rhs=hT[:, k, g * 512:(g + 1) * 512],
                                                start=(k == 0), stop=(k == KT - 1)), r=[t_wc[s]] + t_hT[4 * g:4 * g + 4], w=[t_bank[b]])
                                        cols = slice(bi * TOK + g * 512, bi * TOK + (g + 1) * 512)
                                        drows = slice(row0 + nt * 128, row0 + nt * 128 + M)
                                        if kind == "fm":
                                            if cnt % 2 == 0:
                                                kb.op("act", lambda e, M=M, b=b, st=st: e.activation(out=stg_b[st][0:M, :], in_=bank[b][0:M, :], func=AF.Copy),
                                                      r=[t_bank[b]], w=[t_stg[st]])
                                            else:
                                                kb.op("dve", lambda e, M=M, b=b, st=st: e.tensor_copy(out=stg_b[st][0:M, :], in_=bank[b][0:M, :]),
                                                      r=[t_bank[b]], w=[t_stg[st]])
                                            kb.dma("sp", "st%d" % st, dest[drows, cols], stg_b[st][0:M, :], r=[t_stg[st]], w=[t_abs])
                                        else:
                                            kb.op("act", lambda e, M=M, b=b, st=st: e.activation(out=stg_f[st][0:M, :], in_=bank[b][0:M, :], func=AF.Sigmoid),
                                                  r=[t_bank[b]], w=[t_stg[st]])
                                            kb.dma("sp", "st%d" % st, dest[drows, cols], stg_f[st][0:M, :], r=[t_stg[st]], w=[t_abs])
                            else:
                                for lt in range(NM):
                                    cnt = state["cnt"]
                                    b = 2 + cnt % 4
                                    st = cnt % 4
                                    state["cnt"] += 1
                                    for k in range(KT):
                                        kb.op("pe", lambda e, s=s, lt=lt, b=b, k=k, ncol=ncol: e.matmul(
                                            bank[b][:, 0:ncol], lhsT=hT[:, k, lt * 128:(lt + 1) * 128], rhs=wc[s][:, k, 0:ncol],
                                            start=(k == 0), stop=(k == KT - 1)), r=[t_wc[s], t_hT[lt]], w=[t_bank[b]])
                                    if kind == "tmv":
                                        if cnt % 2 == 0:
                                            kb.op("act", lambda e, b=b, st=st: e.activation(out=stg_b[st][:], in_=bank[b][:], func=AF.Copy),
                                                  r=[t_bank[b]], w=[t_stg[st]])
                                        else:
                                            kb.op("dve", lambda e, b=b, st=st: e.tensor_copy(out=stg_b[st][:], in_=bank[b][:]),
                                                  r=[t_bank[b]], w=[t_stg[st]])
                                        dv = dest.rearrange("(h p) (k d) -> p k h d", p=128, d=128)
                                        kb.dma("sp", "st%d" % st, dv[:, bi * NM + lt, 4 * row0:4 * row0 + 4, :], stg_b[st][:].rearrange("p (h d) -> p h d", d=128),
                                               r=[t_stg[st]], w=[t_abs])
                                    else:
                                        kb.op("dve", lambda e, b=b, st=st, ncol=ncol: e.tensor_copy(out=stg_f[st][:, 0:ncol], in_=bank[b][:, 0:ncol]),
                                              r=[t_bank[b]], w=[t_stg[st]])
                                        kb.dma("sp", "st%d" % st, dest[(bi * NM + lt) * 128:(bi * NM + lt + 1) * 128, :], stg_f[st][:, 0:ncol], r=[t_stg[st]], w=[t_abs])

                ch_all = [(C_KA, 512, "fm", K_abs2, 0), (C_KA + 512, 512, "fm", K_abs2, 512), (C_KI, 64, "fm", kiT_abs, 0),
                          (C_VA, 512, "tmv", V_abs2, 0), (C_VA + 512, 512, "tmv", V_abs2, 1)]
                ch_band = [(C_KB, 512, "fm", KbT_loc, 0), (C_KB + 512, 512, "fm", KbT_loc, 512),
                           (C_VB, 512, "tmv", Vb_loc, 0), (C_VB + 512, 512, "tmv", Vb_loc, 1)]
                ch_own = []
                for i in range(2):
                    ch_own.append((C_QA + 512 * i, 512, "fm", QaT_d, 512 * i))
                for i in range(2):
                    ch_own.append((C_QI + 512 * i, 512, "fm", QiT_d, 512 * i))
                for i in range(2):
                    ch_own.append((C_QB + 512 * i, 512, "fm", QbT_d, 512 * i))
                for i in range(8):
                    ch_own.append((C_GA + 512 * i, 512, "sg", SGT_d, 512 * i))
                ch_own.append((C_WI, 16, "tmw", Wi_d, 0))
                nb_all = 8 if DBG_NBALL is None else DBG_NBALL
                proj_batches(x_own, 1, ch_own)
                proj_batches(x_band, 5, ch_band)
                proj_batches(x_all, nb_all, ch_all)
                kb.barrier()
            if upto <= 1:
                for nm_, src in (("QaT_d", QaT_d), ("K_abs", K_abs[:, :, 0:2048]), ("V_abs", V_abs[:, :, 0:2048]), ("QiT_d", QiT_d), ("kiT_abs", kiT_abs[:, 0:2048]), ("Wi_d", Wi_d), ("SGT_d", SGT_d), ("KbT_loc", KbT_loc[:, 0:2048])):
                    if nm_ in outs:
                        kb.dma("sp", "dbg", outs[nm_], src, w=[Tok()])
                kb.barrier()
                return nc

        t_g = t_abs

        with ExitStack() as p2:
            kiT2 = sb("kiT2", [128, 8192], BF16, p2)
            t_ki = Tok()
            kb.dma("sp", "c2", kiT2[0:64, :], kiT_abs[:, 0:8192], r=[t_abs], w=[t_ki])
            kb.dma("sp", "c2", kiT2[64:128, :], kiT_abs[:, 8192:16384], r=[t_abs], w=[t_ki])
            vis = sb("vis", [128, 1024], F32, p2)
            cbias = sb("cbias", [128, 8], F32, p2)
            kb.dma("sp", "c2", vis[:], vis_in, w=[t_const])
            kb.dma("sp", "c2", cbias[:], cb_in, w=[t_const])
            sc = sb("sc", [128, S], F32, p2)
            maskT = sb("maskT", [128, 128, 128], BF16, p2)
            junk = maskT[:].rearrange("p a b -> p (a b)")
            t_sc = Tok()
            t_maskT = Tok()
            qiT = sb("qiT", [128, 16, 128], BF16, p2)
            wib = sb("wib", [128, 16], F32, p2)
            qaT = sb("qaT", [128, 8, 128], BF16, p2)
            qbT = sb("qbT", [128, 8, 128], BF16, p2)
            t_q = Tok()
            Dg = sb("Dg", [128, 16, 128], BF16, p2)
            t_Dg = Tok()
            Rb = [sb("Rb%d" % i, [128, 512], BF16, p2) for i in range(3)]
            t_Rb = [Tok() for _ in range(3)]
            st = sb("stt", [128, 8], F32, p2)
            predu = sb("predu", [128, 1], U32, p2)
            t_st = Tok()
            mts = [sb("mts%d" % i, [128, 1024], BF16, p2) for i in range(2)]
            t_mts = [Tok(), Tok()]
            kbuf = [sb("kbuf%d" % i, [128, 2048], BF16, p2) for i in range(2)]
            vbuf = [sb("vbuf%d" % i, [128, 16, 129], BF16, p2) for i in range(2)]
            t_kv = [Tok(), Tok()]
            for i in range(2):
                kb.op("pool", lambda e, i=i: e.memset(vbuf[i][:], 1.0), w=[t_kv[i]])
            t5c = [sb("t5c%d" % i, [128, 16, 128], F32, p2) for i in range(2)]
            t_t5c = [Tok(), Tok()]
            Eb = [sb("Eb%d" % i, [128, 4, 128], BF16, p2) for i in range(3)]
            t_Eb = [Tok() for _ in range(3)]
            tmpn = [sb("tmpn%d" % i, [128, 512], F32, p2) for i in range(2)]
            t_tmpn = [Tok(), Tok()]
            ya = sb("ya", [128, 1024], BF16, p2)
            yb = sb("yb", [128, 1024], BF16, p2)
            t_ya = Tok()
            t_yb = Tok()
            yT = [sb("yT%d" % i, [128, 8, 128], BF16, p2) for i in range(2)]
            t_yT = [Tok(), Tok()]
            kbb = sb("kbb", [128, 12, 128], BF16, p2)
            vbb = sb("vbb", [128, 12, 129], BF16, p2)
            t_kvb = Tok()
            kb.op("pool", lambda e: e.memset(vbb[:], 1.0), w=[t_kvb])
            bbc = sb("bbc", [128, 12, 128], F32, p2)
            t_bbc = Tok()

            qiv = QiT_d.rearrange("(hi d) t -> d hi t", d=64)
            qav = QaT_d.rearrange("(h d) t -> d h t", d=128)
            qbv = QbT_d.rearrange("(h d) t -> d h t", d=128)
            kbl = KbT_loc.rearrange("(h d) (k s) -> h d k s", d=128, s=128)
            vbl = Vb_loc.rearrange("(h p) (k d) -> h p k d", p=128, d=128)
            kav = K_abs
            vav = V_abs.rearrange("h p (k d) -> h p k d", d=128)
            ecnt = 0
            kvcnt = 0
            hcnt = 0
            for m in range(NM):
                if upto == 3 and m >= DBG_M:
                    break
                tsl = slice(m * 128, (m + 1) * 128)
                nkt = 8 * m + 8
                Sm = nkt * 128
                kb.dma("sp", "q0", qiT[0:64, :, :], qiv[:, :, tsl], w=[t_q])
                kb.dma("sp", "q0", qiT[64:128, :, :], qiv[:, :, tsl], w=[t_q])
                kb.dma("sp", "q0", wib[:], Wi_d[tsl, :], w=[t_q])
                kb.dma("sp", "q0", qaT[:], qav[:, :, tsl], w=[t_q])
                kb.dma("sp", "q0", qbT[:], qbv[:, :, tsl], w=[t_q])
                for h in range(16):
                    kb.op("pool", lambda e, h=h: e.tensor_scalar(out=Dg[:, h, :], in0=identb[:], scalar1=wib[:, h:h + 1], scalar2=None, op0=ALU.mult),
                          r=[t_q, t_const], w=[t_Dg])
                if P2_STOP <= 1:
                    continue
                nch = Sm // 512
                for c in range(nch):
                    half = 0 if c * 512 < 8192 else 1
                    col0 = c * 512 - 8192 * half
                    ab = 2
                    pr = slice(64 * half, 64 * half + 64)

                    def pmm(h, c=c, pr=pr, col0=col0):
                        kb.op("pe", lambda e: e.matmul(bank[h % 2][:, :], lhsT=qiT[pr, h, :], rhs=kiT2[pr, col0:col0 + 512], start=True, stop=True),
                              r=[t_q, t_ki], w=[t_bank[h % 2]])
                    pmm(0)
                    for h in range(16):
                        if h + 1 < 16:
                            pmm(h + 1)
                        rb = ecnt % 3
                        ecnt += 1
                        kb.op("act", lambda e, h=h, rb=rb: e.activation(out=Rb[rb][:], in_=bank[h % 2][:, :], func=AF.Relu),
                              r=[t_bank[h % 2]], w=[t_Rb[rb]])
                        kb.op("pe", lambda e, h=h, rb=rb, ab=ab: e.matmul(bank[ab][:, :], lhsT=Dg[:, h, :], rhs=Rb[rb][:], start=(h == 0), stop=(h == 15)),
                              r=[t_Dg, t_Rb[rb]], w=[t_bank[ab]])
                    kb.op("dve", lambda e, c=c, ab=ab: e.tensor_copy(out=sc[:, c * 512:(c + 1) * 512], in_=bank[ab][:, :]),
                          r=[t_bank[ab]], w=[t_sc])
                if P2_STOP <= 2:
                    continue
                kb.op("dve", lambda e: e.tensor_reduce(out=st[:, 0:1], in_=sc[:, 0:Sm], axis=AX.X, op=ALU.max), r=[t_sc], w=[t_st])
                kb.op("dve", lambda e: e.tensor_reduce(out=st[:, 1:2], in_=sc[:, 0:Sm], axis=AX.X, op=ALU.min), r=[t_sc], w=[t_st])
                kb.op("dve", lambda e: e.tensor_tensor(out=sc[:, Sm - 1024:Sm], in0=sc[:, Sm - 1024:Sm], in1=vis[:], op=ALU.add),
                      r=[t_sc, t_const, t_st], w=[t_sc])
                kb.op("dve", lambda e: e.tensor_scalar(out=st[:, 3:4], in0=st[:, 1:2], scalar1=-1.0, scalar2=None, op0=ALU.add), r=[t_st], w=[t_st])
                kb.op("dve", lambda e: e.scalar_tensor_tensor(out=st[:, 2:3], in0=st[:, 0:1], scalar=1.0, in1=st[:, 3:4], op0=ALU.add, op1=ALU.subtract),
                      r=[t_st], w=[t_st])
                for it in range(NIT):
                    ci = 2.0 ** (-(it + 1))
                    kb.op("dve", lambda e, ci=ci: e.scalar_tensor_tensor(out=st[:, 4:5], in0=st[:, 2:3], scalar=ci, in1=st[:, 3:4], op0=ALU.mult, op1=ALU.add),
                          r=[t_st], w=[t_st])
                    kb.op("dve", lambda e: e.tensor_scalar(out=junk[:, 0:Sm], in0=sc[:, 0:Sm], scalar1=st[:, 4:5], scalar2=None,
                                                           op0=ALU.is_ge, op1=ALU.add, accum_out=st[:, 5:6]),
                          r=[t_sc, t_st], w=[t_maskT, t_st])
                    kb.op("dve", lambda e: e.tensor_scalar(out=predu[:], in0=st[:, 5:6], scalar1=255.5, scalar2=None, op0=ALU.is_ge), r=[t_st], w=[t_st])
                    kb.op("dve", lambda e: e.copy_predicated(out=st[:, 3:4], mask=predu[:], data=st[:, 4:5]), r=[t_st], w=[t_st])
                if "thr" in outs:
                    kb.dma("sp", "dbg", outs["thr"][:, m:m + 1], st[:, 3:4], r=[t_st], w=[Tok()], allow_slow_non_contiguous=True)
                if P2_STOP <= 3:
                    continue
                for g in range(nkt // 8):
                    ms = g % 2
                    kb.op("dve", lambda e, g=g, ms=ms: e.tensor_scalar(out=mts[ms][:], in0=sc[:, g * 1024:(g + 1) * 1024], scalar1=st[:, 3:4], scalar2=None, op0=ALU.is_ge),
                          r=[t_sc, t_st], w=[t_mts[ms]])
                    for j in range(8):
                        kb.op("pe", lambda e, ms=ms, j=j: e.transpose(bankb[ms][:, j * 128:(j + 1) * 128], mts[ms][:, j * 128:(j + 1) * 128], identb[:]),
                              r=[t_mts[ms], t_const], w=[t_bankb[ms]])
                    kb.op("act", lambda e, g=g, ms=ms: e.activation(out=maskT[:, g * 8:(g + 1) * 8, :].rearrange("p a b -> p (a b)"), in_=bankb[ms][:, :], func=AF.Copy),
                          r=[t_bankb[ms]], w=[t_maskT])
                if P2_STOP <= 4:
                    continue
                for h in range(8):
                    ts5 = hcnt % 2
                    hcnt += 1
                    kb.dma("sp", "t5%d" % ts5, t5c[ts5][:].rearrange("p a b -> p (a b)"), t5c_in[h], w=[t_t5c[ts5]])
                    for kc in range((nkt + 15) // 16):
                        ntl = min(16, nkt - kc * 16)
                        ks = kvcnt % 2
                        kvcnt += 1
                        kb.dma("sp", "kv%d" % ks, kbuf[ks][:, 0:ntl * 128], kav[h, :, kc * 2048:kc * 2048 + ntl * 128], r=[t_abs], w=[t_kv[ks]])
                        kb.dma("sp", "kv%d" % ks, vbuf[ks][:, 0:ntl, 0:128], vav[h, :, kc * 16:kc * 16 + ntl, :], r=[t_abs], w=[t_kv[ks]])
                        for g in range(ntl // 4):
                            lb = 4 + g % 2
                            eb = ecnt % 3
                            ecnt += 1
                            kt0 = kc * 16 + g * 4
                            if DSA_STOP <= 1:
                                continue
                            for j in range(4):
                                kb.op("pe", lambda e, ks=ks, g=g, j=j, lb=lb, h=h: e.matmul(
                                    bank[lb][:, j * 128:(j + 1) * 128], lhsT=kbuf[ks][:, (g * 4 + j) * 128:(g * 4 + j + 1) * 128], rhs=qaT[:, h, :],
                                    start=True, stop=True), r=[t_kv[ks], t_q], w=[t_bank[lb]])
                            if DSA_STOP <= 2:
                                continue
                            nfar = 4 if kt0 < 8 * m - 8 else 0
                            if DSA_VAR == 1:
                                nfar = 4
                            if DSA_VAR == 2:
                                nfar = 0
                            if DSA_VAR == 3 and 0 < nfar < 4:
                                nfar = 0
                            if nfar > 0:
                                kb.op("act", lambda e, lb=lb, eb=eb, nfar=nfar, h=h: e.activation(
                                    out=Eb[eb][:, 0:nfar, :].rearrange("p a b -> p (a b)"), in_=bank[lb][:, 0:nfar * 128], func=AF.Exp,
                                    bias=cbias[:, h:h + 1], scale=SCALE), r=[t_bank[lb], t_const], w=[t_Eb[eb]])
                            if nfar < 4:
                                tn = ecnt % 2
                                c0 = kt0 - (8 * m - 8)
                                nn = 4 - nfar
                                kb.op("dve", lambda e, lb=lb, tn=tn, nfar=nfar, nn=nn: e.tensor_scalar(
                                    out=tmpn[tn][:, 0:nn * 128], in0=bank[lb][:, nfar * 128:512], scalar1=SCALE, scalar2=None, op0=ALU.mult),
                                    r=[t_bank[lb]], w=[t_tmpn[tn]])
                                kb.op("dve", lambda e, tn=tn, nn=nn, c0=c0, ts5=ts5: e.tensor_tensor(
                                    out=tmpn[tn][:, 0:nn * 128], in0=tmpn[tn][:, 0:nn * 128],
                                    in1=t5c[ts5][:, c0:c0 + nn, :].rearrange("p a b -> p (a b)"), op=ALU.add),
                                    r=[t_tmpn[tn], t_t5c[ts5]], w=[t_tmpn[tn]])
                                kb.op("act", lambda e, eb=eb, tn=tn, nfar=nfar, nn=nn: e.activation(
                                    out=Eb[eb][:, nfar:4, :].rearrange("p a b -> p (a b)"), in_=tmpn[tn][:, 0:nn * 128], func=AF.Exp),
                                    r=[t_tmpn[tn]], w=[t_Eb[eb]])
                            if DSA_STOP <= 3:
                                continue
                            kb.op("pool", lambda e, eb=eb, kt0=kt0: e.tensor_tensor(out=Eb[eb][:], in0=Eb[eb][:], in1=maskT[:, kt0:kt0 + 4, :], op=ALU.mult),
                                  r=[t_Eb[eb], t_maskT], w=[t_Eb[eb]])
                            if DSA_STOP <= 4:
                                continue
                            for j in range(4):
                                kt = kt0 + j
                                kb.op("pe", lambda e, eb=eb, j=j, ks=ks, g=g, kt=kt: e.matmul(
                                    bank[3][:, 0:129], lhsT=Eb[eb][:, j, :], rhs=vbuf[ks][:, g * 4 + j, 0:129],
                                    start=(kt == 0), stop=(kt == nkt - 1)), r=[t_Eb[eb], t_kv[ks]], w=[t_bank[3]])
                    if DSA_STOP <= 5:
                        continue
                    kb.op("dve", lambda e: e.reciprocal(out=st[:, 6:7], in_=bank[3][:, 128:129]), r=[t_bank[3]], w=[t_st])
                    kb.op("dve", lambda e, h=h: e.tensor_scalar(out=ya[:, h * 128:(h + 1) * 128], in0=bank[3][:, 0:128], scalar1=st[:, 6:7], scalar2=None, op0=ALU.mult),
                          r=[t_bank[3], t_st], w=[t_ya])
                if P2_STOP <= 5:
                    continue
                for h in range(8):
                    kb.dma("sp", "bb0", bbc[:, 0:5, :].rearrange("p a b -> p (a b)"), bbc_in[0 if m == 0 else 1, h], w=[t_bbc])
                    kb.dma("sp", "bb1", kbb[:, 0:5, :], kbl[h, :, 5 * m:5 * m + 5, :], r=[t_abs], w=[t_kvb])
                    kb.dma("sp", "bb1", vbb[:, 0:5, 0:128], vbl[h, :, 5 * m:5 * m + 5, :], r=[t_abs], w=[t_kvb])
                    for g, (c_0, c_n) in enumerate(((0, 4), (4, 1))):
                        lb = 4 + g % 2
                        eb = ecnt % 3
                        tn = ecnt % 2
                        ecnt += 1
                        for j in range(c_n):
                            kb.op("pe", lambda e, c_0=c_0, j=j, lb=lb, h=h: e.matmul(
                                bank[lb][:, j * 128:(j + 1) * 128], lhsT=kbb[:, c_0 + j, :], rhs=qbT[:, h, :], start=True, stop=True),
                                r=[t_kvb, t_q], w=[t_bank[lb]])
                        kb.op("dve", lambda e, lb=lb, tn=tn, c_n=c_n: e.tensor_scalar(
                            out=tmpn[tn][:, 0:c_n * 128], in0=bank[lb][:, 0:c_n * 128], scalar1=SCALE, scalar2=None, op0=ALU.mult),
                            r=[t_bank[lb]], w=[t_tmpn[tn]])
                        kb.op("dve", lambda e, tn=tn, c_0=c_0, c_n=c_n: e.tensor_tensor(
                            out=tmpn[tn][:, 0:c_n * 128], in0=tmpn[tn][:, 0:c_n * 128], in1=bbc[:, c_0:c_0 + c_n, :].rearrange("p a b -> p (a b)"), op=ALU.add),
                            r=[t_tmpn[tn], t_bbc], w=[t_tmpn[tn]])
                        kb.op("act", lambda e, eb=eb, tn=tn, c_n=c_n: e.activation(out=Eb[eb][:, 0:c_n, :].rearrange("p a b -> p (a b)"), in_=tmpn[tn][:, 0:c_n * 128], func=AF.Exp),
                              r=[t_tmpn[tn]], w=[t_Eb[eb]])
                        for j in range(c_n):
                            c = c_0 + j
                            kb.op("pe", lambda e, eb=eb, j=j, c=c: e.matmul(
                                bank[3][:, 0:129], lhsT=Eb[eb][:, j, :], rhs=vbb[:, c, 0:129], start=(c == 0), stop=(c == 4)),
                                r=[t_Eb[eb], t_kvb], w=[t_bank[3]])
                    kb.op("dve", lambda e: e.reciprocal(out=st[:, 6:7], in_=bank[3][:, 128:129]), r=[t_bank[3]], w=[t_st])
                    kb.op("dve", lambda e, h=h: e.tensor_scalar(out=yb[:, h * 128:(h + 1) * 128], in0=bank[3][:, 0:128], scalar1=st[:, 6:7], scalar2=None, op0=ALU.mult),
                          r=[t_bank[3], t_st], w=[t_yb])
                if P2_STOP <= 6:
                    continue
                for which, (ysrc, tys, ydst) in enumerate(((ya, t_ya, YaT_d), (yb, t_yb, YbT_d))):
                    for h in range(8):
                        kb.op("pe", lambda e, h=h, ysrc=ysrc, which=which: e.transpose(bankb[which][:, h * 128:(h + 1) * 128], ysrc[:, h * 128:(h + 1) * 128], identb[:]),
                              r=[tys, t_const], w=[t_bankb[which]])
                    kb.op("act", lambda e, which=which: e.activation(out=yT[which][:].rearrange("p a b -> p (a b)"), in_=bankb[which][:, :], func=AF.Copy),
                          r=[t_bankb[which]], w=[t_yT[which]])
                    kb.dma("sp", "yt%d" % which, ydst.rearrange("(h d) t -> d h t", d=128)[:, :, tsl], yT[which][:], r=[t_yT[which]], w=[Tok()])
                if "ya" in outs:
                    kb.dma("sp", "dbg", outs["ya"][tsl, :], ya[:], r=[t_ya], w=[Tok()])
                    kb.dma("sp", "dbg", outs["yb"][tsl, :], yb[:], r=[t_yb], w=[Tok()])
                if upto == 3 and m >= DBG_M - 1:
                    break
            kb.barrier()
        if upto <= 3:
            return nc

        def bcast(ap_t, off, n):
            return bass.AP(ap_t.tensor, off, [[0, 128], [1, n]])

        oh1all = sb("oh1all", [128, NM, NE], F32)
        oh2all = sb("oh2all", [128, NM, NE], F32)
        Aall = sb("Aall", [128, NM, NE], BF16)
        wgt = sb("wgt", [128, 2 * NM], F32)
        desti4 = sb("desti4", [128, 8 * NM], I32)
        dest4f = sb("dest4f", [128, 8 * NM], F32)
        t_rt = Tok()
        MT_d = dint("MT_d", [D, TOK])
        with ExitStack() as p3:
            Wua = sb("Wua", [128, 8, D], BF16, p3)
            Wub = sb("Wub", [128, 8, D], BF16, p3)
            t_w3 = Tok()
            kb.dma("pool", "w30", Wua[:], w_up_a.rearrange("(k p) n -> p k n", p=128), w=[t_w3])
            kb.dma("pool", "w30", Wub[:], w_up_b.rearrange("(k p) n -> p k n", p=128), w=[t_w3])
            yaT = sb("yaT", [128, 8, 512], BF16, p3)
            ybT = sb("ybT", [128, 8, 512], BF16, p3)
            t_y3 = Tok()
            mst = [sb("mst%d" % i, [128, 512], BF16, p3) for i in range(2)]
            t_mst = [Tok(), Tok()]
            sga = [sb("sga%d" % i, [128, 512], F32, p3) for i in range(2)]
            sgb = [sb("sgb%d" % i, [128, 512], F32, p3) for i in range(2)]
            t_sg = [Tok(), Tok()]
            tm1 = [sb("tm1%d" % i, [128, 512], F32, p3) for i in range(2)]
            tm2 = [sb("tm2%d" % i, [128, 512], F32, p3) for i in range(2)]
            t_tm = [Tok(), Tok()]
            yav = YaT_d.rearrange("(h d) t -> d h t", d=128)
            ybv = YbT_d.rearrange("(h d) t -> d h t", d=128)
            for g in range(4):
                gs = slice(g * 512, (g + 1) * 512)
                kb.dma("sp", "y30", yaT[:], yav[:, :, gs], w=[t_y3])
                kb.dma("sp", "y30", ybT[:], ybv[:, :, gs], w=[t_y3])
                for nt in range(KT):
                    s_ = nt % 2
                    kb.dma("sp", "sg%d" % s_, sga[s_][:], SGT_d[nt * 128:(nt + 1) * 128, gs], w=[t_sg[s_]])
                    kb.dma("sp", "sg%d" % s_, sgb[s_][:], SGT_d[D + nt * 128:D + (nt + 1) * 128, gs], w=[t_sg[s_]])
                    for h in range(8):
                        kb.op("pe", lambda e, h=h, nt=nt: e.matmul(bank[0][:, :], lhsT=Wua[:, h, nt * 128:(nt + 1) * 128], rhs=yaT[:, h, :], start=(h == 0), stop=(h == 7)),
                              r=[t_w3, t_y3], w=[t_bank[0]])
                    for h in range(8):
                        kb.op("pe", lambda e, h=h, nt=nt: e.matmul(bank[1][:, :], lhsT=Wub[:, h, nt * 128:(nt + 1) * 128], rhs=ybT[:, h, :], start=(h == 0), stop=(h == 7)),
                              r=[t_w3, t_y3], w=[t_bank[1]])
                    kb.op("dve", lambda e, s_=s_: e.tensor_tensor(out=tm1[s_][:], in0=bank[0][:, :], in1=sga[s_][:], op=ALU.mult), r=[t_bank[0], t_sg[s_]], w=[t_tm[s_]])
                    kb.op("dve", lambda e, s_=s_: e.tensor_tensor(out=tm2[s_][:], in0=bank[1][:, :], in1=sgb[s_][:], op=ALU.mult), r=[t_bank[1], t_sg[s_]], w=[t_tm[s_]])
                    kb.op("pool", lambda e, s_=s_, nt=nt: e.tensor_tensor(out=mst[s_][:], in0=tm1[s_][:], in1=tm2[s_][:], op=ALU.add), r=[t_tm[s_]], w=[t_mst[s_]])
                    kb.dma("sp", "ms%d" % s_, MT_d[nt * 128:(nt + 1) * 128, gs], mst[s_][:], r=[t_mst[s_]], w=[Tok()])
            kb.barrier()
        with ExitStack() as p3:
            Wo = sb("Wo", [128, KT, D], BF16, p3)
            Wr = sb("Wr", [128, KT, 72], F32, p3)
            t_w3 = Tok()
            kb.dma("pool", "w31", Wo[:], w_o.rearrange("(k p) n -> p k n", p=128), w=[t_w3])
            kb.dma("sp", "c3", Wr[:], wr_in.rearrange("(k p) n -> p k n", p=128), w=[t_w3])
            g1_b = sb("g1_b", [128, D], F32, p3)
            A2_b = sb("A2_b", [128, D], F32, p3)
            B2_b = sb("B2_b", [128, D], F32, p3)
            br_b = sb("br_b", [128, 72], F32, p3)
            t_c3 = Tok()
            kb.dma("sp", "c3", g1_b[:], bcast(modflat, 2 * D, D), r=[t_modflat], w=[t_c3])
            kb.dma("sp", "c3", A2_b[:], bcast(modflat, 4 * D, D), r=[t_modflat], w=[t_c3])
            kb.dma("sp", "c3", B2_b[:], bcast(modflat, 3 * D, D), r=[t_modflat], w=[t_c3])
            kb.dma("sp", "c3", br_b[:], bcast(br_row, 0, 72), w=[t_c3])
            xt = sb("xt", [128, D], F32, p3)
            t_xt = Tok()
            kb.dma("sp", "c3", xt[:], bcast(nffn_row, 0, D), w=[t_xt])
            kb.op("dve", lambda e: e.scalar_tensor_tensor(out=A2_b[:], in0=A2_b[:], scalar=1.0, in1=xt[:], op0=ALU.add, op1=ALU.mult), r=[t_c3, t_xt], w=[t_c3])
            mT = sb("mT", [128, KT, 512], BF16, p3)
            t_mT = Tok()
            x1 = sb("x1", [128, D], F32, p3)
            h2 = sb("h2", [128, D], F32, p3)
            h2b = sb("h2b", [128, D], BF16, p3)
            h2T = sb("h2T", [128, KT, 128], F32, p3)
            t_x1 = Tok(); t_h2 = Tok(); t_h2b = Tok(); t_h2T = Tok()
            s3 = sb("s3", [128, 16], F32, p3)
            t_s3 = Tok()
            lg = sb("lg", [128, 72], F32, p3)
            mk = sb("mk", [128, 64], F32, p3)
            mk2 = sb("mk2", [128, 64], F32, p3)
            eg = sb("eg", [128, 8], F32, p3)
            ohg = sb("ohg", [128, 8], F32, p3)
            t_lg = Tok()
            epsc3 = sb("epsc3", [128, 1], F32, p3)
            kb.op("dve", lambda e: e.memset(epsc3[:], EPS), w=[t_c3])
            BIG = 1.0e9
            for g in range(4):
                gs = slice(g * 512, (g + 1) * 512)
                kb.dma("sp", "mt0", mT[:], MT_d.rearrange("(k p) t -> p k t", p=128)[:, :, gs], w=[t_mT])
                for lt in range(4):
                    T = g * 4 + lt
                    rows = slice(T * 128, (T + 1) * 128)
                    kb.dma("sp", "x30", xt[:], x_own[rows, :], w=[t_xt])
                    for ch in range(4):
                        b = 2 + ch % 2
                        cs = slice(ch * 512, (ch + 1) * 512)
                        for k in range(KT):
                            kb.op("pe", lambda e, k=k, lt=lt, b=b, cs=cs: e.matmul(bank[b][:, :], lhsT=mT[:, k, lt * 128:(lt + 1) * 128], rhs=Wo[:, k, cs], start=(k == 0), stop=(k == KT - 1)),
                                  r=[t_mT, t_w3], w=[t_bank[b]])
                        kb.op("dve", lambda e, b=b, cs=cs: e.tensor_tensor(out=x1[:, cs], in0=bank[b][:, :], in1=g1_b[:, cs], op=ALU.mult), r=[t_bank[b], t_c3], w=[t_x1])
                    kb.op("pool", lambda e: e.tensor_tensor(out=x1[:], in0=x1[:], in1=xt[:], op=ALU.add), r=[t_x1, t_xt], w=[t_x1])
                    kb.dma("sp", "x31", X1_d[rows, :], x1[:], r=[t_x1], w=[Tok()])
                    kb.op("act", lambda e: e.activation(out=h2[:], in_=x1[:], func=AF.Square, accum_out=s3[:, 0:1]), r=[t_x1], w=[t_h2, t_s3])
                    kb.op("act", lambda e: e.activation(out=s3[:, 0:1], in_=s3[:, 0:1], func=AF.Sqrt, bias=epsc3[:, 0:1], scale=1.0 / D), r=[t_s3, t_c3], w=[t_s3])
                    kb.op("dve", lambda e: e.reciprocal(out=s3[:, 1:2], in_=s3[:, 0:1]), r=[t_s3], w=[t_s3])
                    kb.op("dve", lambda e: e.scalar_tensor_tensor(out=h2[:], in0=x1[:], scalar=s3[:, 1:2], in1=A2_b[:], op0=ALU.mult, op1=ALU.mult), r=[t_x1, t_s3, t_c3], w=[t_h2])
                    kb.op("pool", lambda e: e.tensor_tensor(out=h2[:], in0=h2[:], in1=B2_b[:], op=ALU.add), r=[t_h2, t_c3], w=[t_h2])
                    kb.op("act", lambda e: e.activation(out=h2b[:], in_=h2[:], func=AF.Copy), r=[t_h2], w=[t_h2b])
                    kb.dma("sp", "x32", H2_d[rows, :], h2b[:], r=[t_h2b], w=[Tok()])
                    for q4 in range(4):
                        b = 4 + q4 % 2
                        for j in range(4):
                            k = q4 * 4 + j
                            kb.op("pe", lambda e, b=b, j=j, k=k: e.transpose(bank[b][:, j * 128:(j + 1) * 128], h2[:, k * 128:(k + 1) * 128], identf[:]),
                                  r=[t_h2, t_const], w=[t_bank[b]])
                        kb.op("act", lambda e, b=b, q4=q4: e.activation(out=h2T[:, q4 * 4:(q4 + 1) * 4, :].rearrange("p a b -> p (a b)"), in_=bank[b][:, :], func=AF.Copy),
                              r=[t_bank[b]], w=[t_h2T])
                    for k in range(KT):
                        kb.op("pe", lambda e, k=k: e.matmul(bank[0][:, 0:72], lhsT=h2T[:, k, :], rhs=Wr[:, k, :], start=(k == 0), stop=(k == KT - 1)),
                              r=[t_h2T, t_w3], w=[t_bank[0]])
                    V = lambda fn, r, w: kb.op("dve", fn, r=r, w=w)
                    V(lambda e: e.tensor_tensor(out=lg[:], in0=bank[0][:, 0:72], in1=br_b[:], op=ALU.add), [t_bank[0], t_c3], [t_lg])
                    V(lambda e: e.tensor_reduce(out=s3[:, 2:3], in_=lg[:, 0:8], axis=AX.X, op=ALU.max), [t_lg], [t_s3])
                    V(lambda e: e.tensor_scalar(out=ohg[:], in0=lg[:, 0:8], scalar1=s3[:, 2:3], scalar2=None, op0=ALU.is_equal), [t_lg, t_s3], [t_lg])
                    V(lambda e: e.tensor_scalar(out=s3[:, 3:4], in0=s3[:, 2:3], scalar1=-1.0, scalar2=None, op0=ALU.mult), [t_s3], [t_s3])
                    kb.op("act", lambda e: e.activation(out=eg[:], in_=lg[:, 0:8], func=AF.Exp, bias=s3[:, 3:4], scale=1.0, accum_out=s3[:, 4:5]), r=[t_lg, t_s3], w=[t_lg, t_s3])
                    V(lambda e: e.reciprocal(out=s3[:, 5:6], in_=s3[:, 4:5]), [t_s3], [t_s3])
                    V(lambda e: e.tensor_scalar(out=ohg[:], in0=ohg[:], scalar1=BIG, scalar2=-BIG, op0=ALU.mult, op1=ALU.add), [t_lg], [t_lg])
                    for gg in range(8):
                        V(lambda e, gg=gg: e.tensor_scalar(out=mk[:, gg * 8:(gg + 1) * 8], in0=lg[:, 8 + gg * 8:16 + gg * 8], scalar1=ohg[:, gg:gg + 1], scalar2=None, op0=ALU.add), [t_lg], [t_lg])
                    V(lambda e: e.tensor_reduce(out=s3[:, 6:7], in_=mk[:], axis=AX.X, op=ALU.max), [t_lg], [t_s3])
                    V(lambda e, T=T: e.tensor_scalar(out=oh1all[:, T, :], in0=mk[:], scalar1=s3[:, 6:7], scalar2=None, op0=ALU.is_equal), [t_lg, t_s3], [t_rt])
                    V(lambda e, T=T: e.scalar_tensor_tensor(out=mk2[:], in0=oh1all[:, T, :], scalar=-BIG, in1=mk[:], op0=ALU.mult, op1=ALU.add), [t_rt, t_lg], [t_lg])
                    V(lambda e: e.tensor_reduce(out=s3[:, 7:8], in_=mk2[:], axis=AX.X, op=ALU.max), [t_lg], [t_s3])
                    V(lambda e, T=T: e.tensor_scalar(out=oh2all[:, T, :], in0=mk2[:], scalar1=s3[:, 7:8], scalar2=None, op0=ALU.is_equal), [t_lg, t_s3], [t_rt])
                    V(lambda e, T=T: e.tensor_tensor(out=Aall[:, T, :], in0=oh1all[:, T, :], in1=oh2all[:, T, :], op=ALU.add), [t_rt], [t_rt])
                    V(lambda e: e.tensor_tensor(out=s3[:, 8:9], in0=s3[:, 6:7], in1=s3[:, 7:8], op=ALU.subtract), [t_s3], [t_s3])
                    kb.op("act", lambda e: e.activation(out=s3[:, 9:10], in_=s3[:, 8:9], func=AF.Sigmoid), r=[t_s3], w=[t_s3])
                    V(lambda e, T=T: e.tensor_tensor(out=wgt[:, 2 * T:2 * T + 1], in0=s3[:, 9:10], in1=s3[:, 5:6], op=ALU.mult), [t_s3], [t_rt])
                    V(lambda e, T=T: e.tensor_tensor(out=wgt[:, 2 * T + 1:2 * T + 2], in0=s3[:, 5:6], in1=wgt[:, 2 * T:2 * T + 1], op=ALU.subtract), [t_s3, t_rt], [t_rt])
            kb.barrier()
        if "x1" in outs:
            kb.dma("sp", "dbg", outs["x1"], X1_d, w=[Tok()])
            kb.dma("sp", "dbg", outs["wgt"], wgt[:], r=[t_rt], w=[Tok()])
            kb.dma("sp", "dbg", outs["oh1"], oh1all[:], r=[t_rt], w=[Tok()])
            kb.dma("sp", "dbg", outs["oh2"], oh2all[:], r=[t_rt], w=[Tok()])
            kb.barrier()
        if upto <= 4:
            return nc

        with ExitStack() as p4:
            Um = sb("Um", [128, 128], BF16, p4)
            onesm = sb("onesm", [128, 128], BF16, p4)
            Ue = sb("Ue", [64, 64], F32, p4)
            base13 = sb("base13", [128, 16], F32, p4)
            base2 = sb("base2", [128, 4], F32, p4)
            t_c4 = Tok()
            kb.dma("sp", "c4", Um[:], U_in, w=[t_c4])
            kb.dma("sp", "c4", onesm[:], ones_in, w=[t_c4])
            kb.dma("sp", "c4", Ue[:], Ue_in, w=[t_c4])
            kb.dma("sp", "c4", base13[:], base13_in, w=[t_c4])
            kb.dma("sp", "c4", base2[:], base2_in, w=[t_c4])
            pos = sb("pos", [128, NM, NE], F32, p4)
            cntb = sb("cntb", [128, NE], F32, p4)
            nblk = sb("nblk", [128, NE], F32, p4)
            nbT = sb("nbT", [64, 128], F32, p4)
            pst = sb("pst", [128, NE], F32, p4)
            pend = sb("pend", [128, NE], F32, p4)
            tmp64 = sb("tmp64", [128, NE], F32, p4)
            destf = sb("destf", [128, 2 * NM], F32, p4)
            desti = sb("desti", [128, 2 * NM], I32, p4)
            idx13f = sb("idx13f", [128, 16], F32, p4)
            idx2f = sb("idx2f", [128, 4], F32, p4)
            t_p4 = Tok()
            V = lambda fn, r, w: kb.op("dve", fn, r=r, w=w)
            for T in range(NM):
                kb.op("pe", lambda e, T=T: e.matmul(bank[0][:, 0:NE], lhsT=Um[:], rhs=Aall[:, T, :], start=True, stop=(T == 0)), r=[t_c4, t_rt], w=[t_bank[0]])
                for T2 in range(T):
                    kb.op("pe", lambda e, T2=T2, T=T: e.matmul(bank[0][:, 0:NE], lhsT=onesm[:], rhs=Aall[:, T2, :], start=False, stop=(T2 == T - 1)), r=[t_c4, t_rt], w=[t_bank[0]])
                V(lambda e, T=T: e.tensor_copy(out=pos[:, T, :], in_=bank[0][:, 0:NE]), [t_bank[0]], [t_p4])
            for T in range(NM):
                kb.op("pe", lambda e, T=T: e.matmul(bank[1][:, 0:NE], lhsT=onesm[:], rhs=Aall[:, T, :], start=(T == 0), stop=(T == NM - 1)), r=[t_c4, t_rt], w=[t_bank[1]])
            V(lambda e: e.tensor_copy(out=cntb[:], in_=bank[1][:, 0:NE]), [t_bank[1]], [t_p4])
            V(lambda e: e.memset(nblk[:], 0.0), [], [t_p4])
            for k in range(32):
                V(lambda e, k=k: e.scalar_tensor_tensor(out=nblk[:], in0=cntb[:], scalar=128.0 * k, in1=nblk[:], op0=ALU.is_gt, op1=ALU.add), [t_p4], [t_p4])
            kb.op("pe", lambda e: e.transpose(bank[2][0:64, 0:128], nblk[:, :], identf[:]), r=[t_p4, t_const], w=[t_bank[2]])
            V(lambda e: e.tensor_copy(out=nbT[:], in_=bank[2][0:64, 0:128]), [t_bank[2]], [t_p4])
            kb.op("pe", lambda e: e.matmul(bank[3][:, 0:NE], lhsT=nbT[:], rhs=Ue[:], start=True, stop=True), r=[t_p4, t_c4], w=[t_bank[3]])
            V(lambda e: e.tensor_copy(out=pst[:], in_=bank[3][:, 0:NE]), [t_bank[3]], [t_p4])
            V(lambda e: e.tensor_tensor(out=pend[:], in0=pst[:], in1=nblk[:], op=ALU.add), [t_p4], [t_p4])
            kb.dma("sp", "c4", pst[:], estart_in, w=[t_p4])
            for T in range(NM):
                V(lambda e, T=T: e.tensor_tensor(out=pos[:, T, :], in0=pos[:, T, :], in1=pst[:], op=ALU.add), [t_p4], [t_p4])
                V(lambda e, T=T: e.tensor_tensor(out=tmp64[:], in0=pos[:, T, :], in1=oh1all[:, T, :], op=ALU.mult), [t_p4, t_rt], [t_p4])
                V(lambda e, T=T: e.tensor_reduce(out=destf[:, 2 * T:2 * T + 1], in_=tmp64[:], axis=AX.X, op=ALU.add), [t_p4], [t_p4])
                V(lambda e, T=T: e.tensor_tensor(out=tmp64[:], in0=pos[:, T, :], in1=oh2all[:, T, :], op=ALU.mult), [t_p4, t_rt], [t_p4])
                V(lambda e, T=T: e.tensor_reduce(out=destf[:, 2 * T + 1:2 * T + 2], in_=tmp64[:], axis=AX.X, op=ALU.add), [t_p4], [t_p4])
            V(lambda e: e.tensor_copy(out=desti[:], in_=destf[:]), [t_p4], [t_p4])
            for cpi in range(4):
                V(lambda e, cpi=cpi: e.tensor_scalar(out=dest4f[:].rearrange("p (a c) -> p a c", c=4)[:, :, cpi], in0=destf[:], scalar1=4.0, scalar2=float(cpi), op0=ALU.mult, op1=ALU.add), [t_p4], [t_p4])
            V(lambda e: e.tensor_copy(out=desti4[:], in_=dest4f[:]), [t_p4], [t_p4])
            if "desti" in outs:
                kb.dma("sp", "dbg", outs["desti"], desti[:], r=[t_p4], w=[Tok()])
                kb.dma("sp", "dbg", outs["pst"], pst[:], r=[t_p4], w=[Tok()])
                kb.dma("sp", "dbg", outs["nblk"], nblk[:], r=[t_p4], w=[Tok()])
            if upto <= 5:
                kb.barrier()
                return nc
            hb = [sb("hb0", [128, D], BF16, p4)] * 2
            t_hb = [Tok()] * 2
            t_xd = Tok()
            zt = sb("zt", [128, D], BF16, p4)
            t_zt = Tok()
            kb.op("dve", lambda e: e.memset(zt[:], 0.0), w=[t_zt])
            for k in range(NBLK):
                kb.dma("sp", "c4", Xd[k * 128:(k + 1) * 128, :], zt[:], r=[t_zt], w=[t_xd])
            for T in range(NM):
                s_ = T % 2
                kb.dma("sp", "hb%d" % s_, hb[s_][:], H2_d[T * 128:(T + 1) * 128, :], w=[t_hb[s_]])
                for j in range(2):
                    kb.custom("pool", "sc0", lambda e, s_=s_, T=T, j=j: e.indirect_dma_start(
                        out=Xd, out_offset=bass.IndirectOffsetOnAxis(ap=desti[:, 2 * T + j:2 * T + j + 1], axis=0), in_=hb[s_][:], in_offset=None),
                        16, r=[t_hb[s_], t_p4], w=[t_xd])
            kb.barrier()
            if "Xd" in outs:
                kb.dma("sp", "dbg", outs["Xd"], Xd[0:2048, :], w=[Tok()])
            if upto <= 6:
                kb.barrier()
                return nc
            w1e = [sb("w1e%d" % i, [128, KT, 512], BF16, p4) for i in range(2)]
            w3e = [sb("w3e%d" % i, [128, KT, 512], BF16, p4) for i in range(2)]
            w2e = [sb("w2e%d" % i, [128, 4, D], BF16, p4) for i in range(2)]
            t_we = [Tok(), Tok()]
            xblk = [sb("xblk%d" % i, [128, D], BF16, p4) for i in range(2)]
            t_xblk = [Tok(), Tok()]
            xTb = sb("xTb", [128, KT, 128], BF16, p4)
            t_xTb = Tok()
            s1 = sb("s1", [128, 512], F32, p4)
            ab = sb("ab", [128, 512], BF16, p4)
            aT = sb("aT", [128, 4, 128], BF16, p4)
            t_s1 = Tok(); t_ab = Tok(); t_aT = Tok()
            yblk = [sb("yblk%d" % i, [128, D], F32, p4) for i in range(2)]
            t_yblk = [Tok(), Tok()]
            t_yd = Tok()
            for k in range(NBLK if upto > 7 else 2 * CAP):
                ex = k // CAP
                sw = ex % 2
                s_ = k % 2
                if k % CAP == 0:
                    kb.dma("pool", "we%d" % sw, w1e[sw][:], w1_in[ex * D:(ex + 1) * D, :].rearrange("(k p) f -> p k f", p=128), w=[t_we[sw]])
                    kb.dma("pool", "we%d" % sw, w3e[sw][:], w3_in[ex * D:(ex + 1) * D, :].rearrange("(k p) f -> p k f", p=128), w=[t_we[sw]])
                    kb.dma("pool", "we%d" % sw, w2e[sw][:], w2_in[ex * 512:(ex + 1) * 512, :].rearrange("(k p) f -> p k f", p=128), w=[t_we[sw]])
                kb.dma("sp", "xk%d" % s_, xblk[s_][:], Xd[k * 128:(k + 1) * 128, :], w=[t_xblk[s_]])
                for half in range(2):
                    for j in range(8):
                        kk = half * 8 + j
                        kb.op("pe", lambda e, s_=s_, half=half, j=j, kk=kk: e.transpose(bankb[half][:, j * 128:(j + 1) * 128], xblk[s_][:, kk * 128:(kk + 1) * 128], identb[:]),
                              r=[t_xblk[s_], t_const], w=[t_bankb[half]])
                    kb.op("act", lambda e, half=half: e.activation(out=xTb[:, half * 8:(half + 1) * 8, :].rearrange("p a b -> p (a b)"), in_=bankb[half][:, :], func=AF.Copy),
                          r=[t_bankb[half]], w=[t_xTb])
                for kk in range(KT):
                    kb.op("pe", lambda e, sw=sw, kk=kk: e.matmul(bank[0][:, :], lhsT=xTb[:, kk, :], rhs=w1e[sw][:, kk, :], start=(kk == 0), stop=(kk == KT - 1)),
                          r=[t_xTb, t_we[sw]], w=[t_bank[0]])
                for kk in range(KT):
                    kb.op("pe", lambda e, sw=sw, kk=kk: e.matmul(bank[1][:, :], lhsT=xTb[:, kk, :], rhs=w3e[sw][:, kk, :], start=(kk == 0), stop=(kk == KT - 1)),
                          r=[t_xTb, t_we[sw]], w=[t_bank[1]])
                kb.op("act", lambda e: e.activation(out=s1[:], in_=bank[0][:, :], func=AF.Silu), r=[t_bank[0]], w=[t_s1])
                kb.op("dve", lambda e: e.tensor_tensor(out=ab[:], in0=bank[1][:, :], in1=s1[:], op=ALU.mult), r=[t_bank[1], t_s1], w=[t_ab])
                for f in range(4):
                    kb.op("pe", lambda e, f=f: e.transpose(bankb[0][:, f * 128:(f + 1) * 128], ab[:, f * 128:(f + 1) * 128], identb[:]), r=[t_ab, t_const], w=[t_bankb[0]])
                kb.op("act", lambda e: e.activation(out=aT[:].rearrange("p a b -> p (a b)"), in_=bankb[0][:, 0:512], func=AF.Copy), r=[t_bankb[0]], w=[t_aT])
                for ch in range(4):
                    b = 2 + ch
                    for f in range(4):
                        kb.op("pe", lambda e, sw=sw, ch=ch, f=f, b=b: e.matmul(bank[b][:, :], lhsT=aT[:, f, :], rhs=w2e[sw][:, f, ch * 512:(ch + 1) * 512], start=(f == 0), stop=(f == 3)),
                              r=[t_aT, t_we[sw]], w=[t_bank[b]])
                    if ch % 2 == 0:
                        kb.op("dve", lambda e, s_=s_, ch=ch, b=b: e.tensor_copy(out=yblk[s_][:, ch * 512:(ch + 1) * 512], in_=bank[b][:, :]), r=[t_bank[b]], w=[t_yblk[s_]])
                    else:
                        kb.op("act", lambda e, s_=s_, ch=ch, b=b: e.activation(out=yblk[s_][:, ch * 512:(ch + 1) * 512], in_=bank[b][:, :], func=AF.Copy), r=[t_bank[b]], w=[t_yblk[s_]])
                kb.dma("sp", "yk%d" % s_, Yd[k * 128:(k + 1) * 128, :], yblk[s_][:], r=[t_yblk[s_]], w=[t_yd])
            kb.barrier()
        if "Yd" in outs:
            kb.dma("sp", "dbg", outs["Yd"], Yd[0:256, :], w=[Tok()])
        if upto <= 7:
            kb.barrier()
            return nc
        with ExitStack() as p5:
            g2_b = sb("g2_b", [128, D], F32, p5)
            nfin_b = sb("nfin_b", [128, D], F32, p5)
            t_c5 = Tok()
            kb.dma("sp", "c5", g2_b[:], bcast(modflat, 5 * D, D), r=[t_modflat], w=[t_c5])
            kb.dma("sp", "c5", nfin_b[:], bcast(nfin_row, 0, D), w=[t_c5])
            y1 = [sb("y1%d" % i, [128, D], F32, p5) for i in range(2)]
            y2 = [sb("y2%d" % i, [128, D], F32, p5) for i in range(2)]
            xr = [sb("xr%d" % i, [128, D], F32, p5) for i in range(2)]
            ot = [sb("ot%d" % i, [128, D], F32, p5) for i in range(2)]
            jk = sb("jk", [128, D], BF16, p5)
            s5 = sb("s5", [128, 8], F32, p5)
            eps5 = sb("eps5", [128, 1], F32, p5)
            t_y12 = [Tok(), Tok()]; t_xr = [Tok(), Tok()]; t_ot = [Tok(), Tok()]; t_s5 = Tok(); t_jk = Tok()
            kb.op("dve", lambda e: e.memset(eps5[:], EPS), w=[t_s5])
            for T in range(NM):
                s_ = T % 2
                rows = slice(T * 128, (T + 1) * 128)
                for cpi in range(4):
                    kb.custom("pool", "ga%d" % s_, lambda e, s_=s_, T=T, cpi=cpi: e.indirect_dma_start(
                        out=y1[s_][:, cpi * 512:(cpi + 1) * 512], out_offset=None, in_=Yd4, in_offset=bass.IndirectOffsetOnAxis(ap=desti4[:, (2 * T) * 4 + cpi:(2 * T) * 4 + cpi + 1], axis=0)), 16, r=[t_p4], w=[t_y12[s_]])
                    kb.custom("pool", "ga%d" % s_, lambda e, s_=s_, T=T, cpi=cpi: e.indirect_dma_start(
                        out=y2[s_][:, cpi * 512:(cpi + 1) * 512], out_offset=None, in_=Yd4, in_offset=bass.IndirectOffsetOnAxis(ap=desti4[:, (2 * T + 1) * 4 + cpi:(2 * T + 1) * 4 + cpi + 1], axis=0)), 16, r=[t_p4], w=[t_y12[s_]])
                kb.dma("sp", "xr%d" % s_, xr[s_][:], X1_d[rows, :], w=[t_xr[s_]])
                kb.op("dve", lambda e, s_=s_, T=T: e.tensor_scalar(out=y1[s_][:], in0=y1[s_][:], scalar1=wgt[:, 2 * T:2 * T + 1], scalar2=None, op0=ALU.mult), r=[t_y12[s_], t_rt], w=[t_y12[s_]])
                kb.op("dve", lambda e, s_=s_, T=T: e.scalar_tensor_tensor(out=y1[s_][:], in0=y2[s_][:], scalar=wgt[:, 2 * T + 1:2 * T + 2], in1=y1[s_][:], op0=ALU.mult, op1=ALU.add), r=[t_y12[s_], t_rt], w=[t_y12[s_]])
                kb.op("pool", lambda e, s_=s_: e.tensor_tensor(out=y1[s_][:], in0=y1[s_][:], in1=g2_b[:], op=ALU.mult), r=[t_y12[s_], t_c5], w=[t_y12[s_]])
                kb.op("pool", lambda e, s_=s_: e.tensor_tensor(out=xr[s_][:], in0=xr[s_][:], in1=y1[s_][:], op=ALU.add), r=[t_y12[s_], t_xr[s_]], w=[t_xr[s_]])
                kb.op("act", lambda e, s_=s_: e.activation(out=jk[:], in_=xr[s_][:], func=AF.Square, accum_out=s5[:, 0:1]), r=[t_xr[s_]], w=[t_jk, t_s5])
                kb.op("act", lambda e: e.activation(out=s5[:, 0:1], in_=s5[:, 0:1], func=AF.Sqrt, bias=eps5[:, 0:1], scale=1.0 / D), r=[t_s5], w=[t_s5])
                kb.op("dve", lambda e: e.reciprocal(out=s5[:, 1:2], in_=s5[:, 0:1]), r=[t_s5], w=[t_s5])
                kb.op("dve", lambda e, s_=s_: e.scalar_tensor_tensor(out=ot[s_][:], in0=xr[s_][:], scalar=s5[:, 1:2], in1=nfin_b[:], op0=ALU.mult, op1=ALU.mult), r=[t_xr[s_], t_s5, t_c5], w=[t_ot[s_]])
                kb.dma("sp", "ot%d" % s_, y_out[rows, :], ot[s_][:], r=[t_ot[s_]], w=[Tok()])
            kb.barrier()
        return nc


DBG_M = 2
P2_STOP = 99
DSA_STOP = 99
DSA_VAR = 0
DBG_NBALL = None


def t5_bucket_np(rel):
    nb = 16
    ret = (rel > 0).astype(np.int32) * nb
    n = np.abs(rel)
    max_exact = nb // 2
    nf = np.maximum(n, 1).astype(np.float32)
    large = max_exact + (np.log(nf / np.float32(max_exact)) / np.float32(math.log(1024 / max_exact)) * np.float32(nb - max_exact)).astype(np.int32)
    large = np.minimum(large, nb - 1)
    return ret + np.where(n < max_exact, n, large)


def make_inputs(inp):
    f32 = np.float32
    x = np.asarray(inp["x"], f32)[0]
    col = lambda v: np.ascontiguousarray(np.asarray(v, f32).reshape(-1, 128).T)
    t5 = np.asarray(inp["t5_table"], f32)
    rel_t = np.asarray(inp["rel_table"], f32)[0]
    sl = np.arange(128)[:, None]
    tl = np.arange(128)[None, :]
    common = {
        "ccol": col(inp["c"][0]), "w_ada": np.asarray(inp["w_ada"], f32)[0], "bada_col": col(inp["b_ada"][0]),
        "nmix_col": col(inp["norm_mix"][0]), "w_in": np.asarray(inp["w_in"], f32)[0],
        "ident_f": np.eye(128, dtype=f32), "ident_b": np.eye(128, dtype=f32).astype(ml_dtypes.bfloat16),
        "cb_in": np.ascontiguousarray(np.broadcast_to(t5[15][None, :], (128, 8))).astype(f32),
        "w_up_a": np.asarray(inp["w_up_a"], f32)[0], "w_up_b": np.asarray(inp["w_up_b"], f32)[0], "w_o": np.asarray(inp["w_o"], f32)[0],
        "nffn_row": np.asarray(inp["norm_ffn"], f32).reshape(1, D), "nfin_row": np.asarray(inp["norm_final"], f32).reshape(1, D),
        "wr_in": np.ascontiguousarray(np.concatenate([np.asarray(inp["w_rg"], f32)[0], np.asarray(inp["w_re"], f32)[0]], axis=1)),
        "br_row": np.concatenate([np.asarray(inp["b_rg"], f32)[0], np.asarray(inp["b_re"], f32)[0]]).reshape(1, 72),
        "w1_in": np.asarray(inp["w1"], f32)[0].reshape(NE * D, 512), "w3_in": np.asarray(inp["w3"], f32)[0].reshape(NE * D, 512),
        "w2_in": np.asarray(inp["w2"], f32)[0].reshape(NE * 512, D),
        "U_in": np.triu(np.ones((128, 128), f32), 1).astype(ml_dtypes.bfloat16), "ones_in": np.ones((128, 128), f32).astype(ml_dtypes.bfloat16),
        "Ue_in": np.triu(np.ones((64, 64), f32), 1),
        "base13_in": (np.arange(16)[None, :] * 128 + np.arange(128)[:, None]).astype(f32),
        "base2_in": (np.arange(4)[None, :] * 128 + np.arange(128)[:, None]).astype(f32),
        "estart_in": np.ascontiguousarray(np.broadcast_to((np.arange(NE) * CAP * 128).astype(f32)[None, :], (128, NE))),
    }
    maps = []
    for r in range(NCORE):
        d = dict(common)
        d["x_own"] = np.ascontiguousarray(x.reshape(NM, NCORE, 128, D)[:, r].reshape(TOK, D))
        vis = np.zeros((128, 8, 128), f32)
        for c in range(8):
            if c > r:
                vis[:, c, :] = -1e30
            elif c == r:
                vis[:, c, :] = np.where((np.arange(128)[None, :] < 64) | (np.arange(128)[:, None] >= 64), 0.0, -1e30)
        d["vis_in"] = vis.reshape(128, 1024)
        t5c = np.zeros((8, 128, 16, 128), f32)
        for c in range(16):
            dd = c - 8 - r
            if dd > 0:
                continue
            rel = 128 * dd + sl - tl
            bk = t5_bucket_np(rel.astype(np.int32))
            t5c[:, :, c, :] = np.transpose(t5[bk], (2, 0, 1))
        d["t5c_in"] = t5c.reshape(8, 128, 16 * 128)
        xt = x.reshape(128, 128, D)
        xband = np.zeros((NM, 5, 128, D), f32)
        bbc = np.full((2, 8, 128, 5, 128), NEG, f32)
        for c in range(5):
            dd = c - 4
            dch = -2 * dd + (tl >= 64).astype(np.int32) - (sl >= 64).astype(np.int32)
            valid = (dch >= 0) & (dch <= 8)
            tms = -(128 * dd + sl - tl)
            idx = np.clip(tms, -128, 128) + 128
            vals = rel_t[:, idx]
            bbc[1, :, :, c, :] = np.where(valid[None], vals, NEG)
            if r - 4 + c >= 0:
                bbc[0, :, :, c, :] = bbc[1, :, :, c, :]
            for m in range(NM):
                kt = 8 * m + r - 4 + c
                if kt >= 0:
                    xband[m, c] = xt[kt]
        d["bbc_in"] = bbc.reshape(2, 8, 128, 5 * 128)
        d["x_band"] = xband.reshape(80 * 128, D)
        d["x_all"] = x
        maps.append(d)
    return maps


def kernel(**inputs):
    nc = build(99, ())
    maps = make_inputs(inputs)
    res = run_bass_kernel_spmd(nc, maps, core_ids=list(range(NCORE)))
    out = np.zeros((NM, NCORE, 128, D), np.float32)
    for r in range(NCORE):
        out[:, r] = np.asarray(res.results[r]["y_out"], np.float32).reshape(NM, 128, D)
    return out.reshape(1, S, D)
```
